# Optimizing a Trainium2 kernel written in Bass

```python
import math
import jax
import jax.numpy as jnp
from jax import lax
import numpy as np

D_MODEL = 1024
BATCH = 8
SEQ = 4096
DEPTH = 2

N_EVEN = (DEPTH + 1) // 2
N_ODD = DEPTH // 2
EPS = 1e-6
CONV_W = 3

HG_HEADS = 4
HG_DK = 128
HG_DV = 128
HG_WIDTH = HG_HEADS * HG_DK
HG_CHUNK = 64
SC_WIDTH = D_MODEL - HG_WIDTH
SC_GROUPS = 8
AB_SIZES = (HG_WIDTH,) * 4 + (SC_WIDTH,) * 3
AB_IN = sum(AB_SIZES)

NSA_HEADS = 16
NSA_KV = 4
NSA_HPG = NSA_HEADS // NSA_KV
NSA_DH = D_MODEL // NSA_HEADS
CMP_LEN = 32
CMP_STRIDE = 16
CMP_HIDDEN = 256
SEL_BLOCK = 64
SEL_TOPN = 16
WINDOW = 512
NSA_QB = 64
NSA_SIZES = (NSA_HEADS * NSA_DH, 3 * NSA_HEADS) + (NSA_KV * NSA_DH,) * 6
NSA_IN = sum(NSA_SIZES)

REL_BUCKETS = 32
REL_MAX_DIST = 1024

D_FF = 2816

kernel_name = 'hybrid_hgrn2_shortconv_nsa_convffn_adaln'


def split_cols(a, sizes):
    return jnp.split(a, [int(v) for v in np.cumsum(sizes)[:-1]], axis=-1)


def rmsnorm(x, g):
    xf = x.astype(jnp.float32)
    y = xf * lax.rsqrt(jnp.mean(xf * xf, axis=-1, keepdims=True) + EPS)
    return (y * g.astype(jnp.float32)).astype(x.dtype)


def causal_dwconv3(u, w):
    S = u.shape[1]
    up = jnp.pad(u, ((0, 0), (CONV_W - 1, 0), (0, 0)))
    return up[:, 0:S] * w[0] + up[:, 1:S + 1] * w[1] + up[:, 2:S + 2] * w[2]


def rel_bucket(dist):
    n = jnp.maximum(dist, 0)
    exact = REL_BUCKETS // 2
    large = exact + (jnp.log(jnp.maximum(n, exact).astype(jnp.float32) / exact)
                     / math.log(REL_MAX_DIST / exact) * (REL_BUCKETS - exact)).astype(jnp.int32)
    return jnp.where(n < exact, n, jnp.minimum(large, REL_BUCKETS - 1))


def masked_softmax(logits, mask):
    logits = jnp.where(mask, logits.astype(jnp.float32), -jnp.inf)
    m = jnp.max(logits, axis=-1, keepdims=True)
    m = jnp.where(jnp.isfinite(m), m, 0.0)
    e = jnp.exp(logits - m)
    den = jnp.sum(e, axis=-1, keepdims=True)
    return e / jnp.where(den > 0, den, 1.0)


def hgrn2_chunked(q, k, v, log_f):
    f32 = jnp.float32
    q, k, v, log_f = (a.astype(f32) for a in (q, k, v, log_f))
    B, H, S, K = q.shape
    V = v.shape[-1]
    C = HG_CHUNK
    NC = S // C

    def to_chunks(a):
        return jnp.moveaxis(a.reshape(B, H, NC, C, a.shape[-1]), 2, 0)

    causal = jnp.tril(jnp.ones((C, C), bool))[:, :, None]

    def step(state, inp):
        qi, ki, vi, gi = inp
        b = jnp.cumsum(gi, axis=2)
        diff = b[:, :, :, None, :] - b[:, :, None, :, :]
        decay = jnp.exp(jnp.where(causal, diff, -jnp.inf))
        attn = jnp.einsum('bhtk,bhsk,bhtsk->bhts', qi, ki, decay)
        o = (jnp.einsum('bhts,bhsv->bhtv', attn, vi)
             + jnp.einsum('bhtk,bhkv->bhtv', qi * jnp.exp(b), state))
        b_last = b[:, :, -1]
        state = (state * jnp.exp(b_last)[..., None]
                 + jnp.einsum('bhsk,bhsv->bhkv', ki * jnp.exp(b_last[:, :, None] - b), vi))
        return state, o

    state0 = jnp.zeros((B, H, K, V), f32)
    _, o = lax.scan(step, state0, (to_chunks(q), to_chunks(k), to_chunks(v), to_chunks(log_f)))
    return jnp.moveaxis(o, 0, 2).reshape(B, H, S, V)


def mixer_hgrn2_shortconv(h, w_in, w_out, lower_bound, onorm_g, sconv_w):
    B, S, _ = h.shape
    f32 = jnp.float32
    q, f, i, g, gb, gc, sv = split_cols(h @ w_in, AB_SIZES)
    lb = lower_bound.astype(f32)
    fgate = lb + (1.0 - lb) * jax.nn.sigmoid(f.astype(f32))

    def heads(a):
        return a.reshape(B, S, HG_HEADS, -1).transpose(0, 2, 1, 3)

    o = hgrn2_chunked(heads(q), heads(1.0 - fgate), heads(i), heads(jnp.log(fgate)))
    o = rmsnorm(o, onorm_g).transpose(0, 2, 1, 3).reshape(B, S, HG_WIDTH)
    o_a = (o * jax.nn.silu(g.astype(f32))).astype(h.dtype)
    o_b = gb * causal_dwconv3(gc * sv, sconv_w)
    return jnp.concatenate([o_a, o_b.astype(h.dtype)], axis=-1) @ w_out


def mixer_nsa(h, w_in, w_out, pos_k, pos_v, w1_k, w2_k, w1_v, w2_v, rel_bias):
    B, S, _ = h.shape
    f32 = jnp.float32
    q, gl, kc, vc, ks, vs, kw, vw = split_cols(h @ w_in, NSA_SIZES)
    q = q.reshape(B, S, NSA_KV, NSA_HPG, NSA_DH).transpose(0, 2, 3, 1, 4) * (NSA_DH ** -0.5)
    gates = jax.nn.sigmoid(gl.astype(f32)).reshape(B, S, 3, NSA_KV, NSA_HPG).transpose(2, 0, 3, 4, 1)

    def kvh(a):
        return a.reshape(B, S, NSA_KV, NSA_DH).transpose(0, 2, 1, 3)

    kc, vc, ks, vs, kw, vw = (kvh(a) for a in (kc, vc, ks, vs, kw, vw))

    n_cmp = (S - CMP_LEN) // CMP_STRIDE + 1
    cmp_idx = np.arange(n_cmp)[:, None] * CMP_STRIDE + np.arange(CMP_LEN)[None, :]
    cmp_end = jnp.asarray(cmp_idx[:, -1], jnp.int32)

    def compress(a, pos, w1, w2):
        blocks = (a[:, :, cmp_idx] + pos).reshape(B, NSA_KV, n_cmp, CMP_LEN * NSA_DH)
        return jax.nn.silu(blocks @ w1) @ w2

    k_cmp = compress(kc, pos_k, w1_k, w2_k)
    v_cmp = compress(vc, pos_v, w1_v, w2_v).astype(f32)

    n_sel = S // SEL_BLOCK
    n_top = min(SEL_TOPN, n_sel)
    c_start = np.arange(n_cmp)[:, None] * CMP_STRIDE
    s_start = np.arange(n_sel)[None, :] * SEL_BLOCK
    inside = np.clip(np.minimum(c_start + CMP_LEN, s_start + SEL_BLOCK) - np.maximum(c_start, s_start), 0, None)
    cmp_to_sel = jnp.asarray(inside / CMP_LEN, f32)
    blk = jnp.arange(n_sel)

    kw_pad = jnp.pad(kw, ((0, 0), (0, 0), (WINDOW, 0), (0, 0)))
    vw_pad = jnp.pad(vw, ((0, 0), (0, 0), (WINDOW, 0), (0, 0)))
    rb = rel_bias.astype(f32)
    rb_grp = rb.reshape(REL_BUCKETS, NSA_KV, NSA_HPG)
    gather = jax.vmap(jax.vmap(lambda a, p: a[p]))

    def shared_bias(dist):
        return rb[rel_bucket(dist)].reshape(*dist.shape, NSA_KV, NSA_HPG).transpose(2, 3, 0, 1)

    def query_block(start):
        t = start + jnp.arange(NSA_QB)
        qb = lax.dynamic_slice_in_dim(q, start, NSA_QB, axis=3)
        gb = lax.dynamic_slice_in_dim(gates, start, NSA_QB, axis=4)[..., None]
        dist_c = t[:, None] - cmp_end[None, :]
        s_c = jnp.einsum('bgiqd,bgnd->bgiqn', qb, k_cmp).astype(f32) + shared_bias(dist_c)
        p_c = masked_softmax(s_c, dist_c >= 0)
        o_c = jnp.einsum('bgiqn,bgnd->bgiqd', p_c, v_cmp)
        imp = jnp.einsum('bgiqn,ns->bgqs', p_c, cmp_to_sel)
        cur = (t // SEL_BLOCK)[:, None]
        forced = (blk[None] == 0) | (blk[None] == cur) | (blk[None] == cur - 1)
        score = jnp.where(blk[None] > cur, -jnp.inf, jnp.where(forced, jnp.inf, imp))
        _, sel = lax.top_k(score, n_top)
        pos = (sel[..., None] * SEL_BLOCK + jnp.arange(SEL_BLOCK)).reshape(B, NSA_KV, NSA_QB, n_top * SEL_BLOCK)
        k_sel = gather(ks, pos)
        v_sel = gather(vs, pos).astype(f32)
        dist_s = t[:, None] - pos
        bias_s = jnp.moveaxis(rb_grp[rel_bucket(dist_s), jnp.arange(NSA_KV)[None, :, None, None]], -1, 2)
        s_s = jnp.einsum('bgiqd,bgqkd->bgiqk', qb, k_sel).astype(f32) + bias_s
        p_s = masked_softmax(s_s, (dist_s >= 0)[:, :, None])
        o_s = jnp.einsum('bgiqk,bgqkd->bgiqd', p_s, v_sel)
        kwb = lax.dynamic_slice_in_dim(kw_pad, start, WINDOW + NSA_QB, axis=2)
        vwb = lax.dynamic_slice_in_dim(vw_pad, start, WINDOW + NSA_QB, axis=2).astype(f32)
        wpos = start - WINDOW + jnp.arange(WINDOW + NSA_QB)
        dist_w = t[:, None] - wpos[None, :]
        mask_w = (dist_w >= 0) & (dist_w < WINDOW) & (wpos[None, :] >= 0)
        s_w = jnp.einsum('bgiqd,bgkd->bgiqk', qb, kwb).astype(f32) + shared_bias(dist_w)
        p_w = masked_softmax(s_w, mask_w)
        o_w = jnp.einsum('bgiqk,bgkd->bgiqd', p_w, vwb)
        return gb[0] * o_c + gb[1] * o_s + gb[2] * o_w

    starts = jnp.arange(S // NSA_QB, dtype=jnp.int32) * NSA_QB
    o = lax.map(query_block, starts)
    o = o.transpose(1, 0, 4, 2, 3, 5).reshape(B, S, NSA_HEADS * NSA_DH)
    return o.astype(h.dtype) @ w_out


def conv_ffn(h, w_up, conv_w, w_down):
    gate, up = jnp.split(h @ w_up, 2, axis=-1)
    return (jax.nn.silu(causal_dwconv3(gate, conv_w)) * up) @ w_down


def setup_inputs(seed: int = 0) -> dict:
    key = jax.random.key(seed)
    keys = iter(jax.random.split(key, 40))

    def nrm(shape, scale):
        return jax.random.normal(next(keys), shape, jnp.float32) * scale

    D = D_MODEL
    return {
        'x': nrm((BATCH, SEQ, D), 1.0),
        'c': nrm((BATCH, D), 1.0),
        'mod_w': nrm((DEPTH, D, 6 * D), 0.5 * D ** -0.5),
        'mod_b': nrm((DEPTH, 6 * D), 0.02),
        'norm_mix_g': 1.0 + nrm((DEPTH, D), 0.02),
        'norm_ffn_g': 1.0 + nrm((DEPTH, D), 0.02),
        'ab_w_in': nrm((N_EVEN, D, AB_IN), D ** -0.5),
        'ab_w_out': nrm((N_EVEN, HG_WIDTH + SC_WIDTH, D), (HG_WIDTH + SC_WIDTH) ** -0.5),
        'hgrn_lb_logits': nrm((N_EVEN + 1, HG_WIDTH), 0.1),
        'hgrn_onorm_g': 1.0 + nrm((N_EVEN, HG_DV), 0.02),
        'sconv_w': nrm((N_EVEN, CONV_W, SC_WIDTH), CONV_W ** -0.5),
        'nsa_w_in': nrm((N_ODD, D, NSA_IN), D ** -0.5),
        'nsa_w_out': nrm((N_ODD, NSA_HEADS * NSA_DH, D), (NSA_HEADS * NSA_DH) ** -0.5),
        'nsa_cmp_pos_k': nrm((N_ODD, CMP_LEN, NSA_DH), 0.02),
        'nsa_cmp_pos_v': nrm((N_ODD, CMP_LEN, NSA_DH), 0.02),
        'nsa_cmp_w1_k': nrm((N_ODD, CMP_LEN * NSA_DH, CMP_HIDDEN), (CMP_LEN * NSA_DH) ** -0.5),
        'nsa_cmp_w2_k': nrm((N_ODD, CMP_HIDDEN, NSA_DH), CMP_HIDDEN ** -0.5),
        'nsa_cmp_w1_v': nrm((N_ODD, CMP_LEN * NSA_DH, CMP_HIDDEN), (CMP_LEN * NSA_DH) ** -0.5),
        'nsa_cmp_w2_v': nrm((N_ODD, CMP_HIDDEN, NSA_DH), CMP_HIDDEN ** -0.5),
        'rel_bias': nrm((REL_BUCKETS, NSA_HEADS), 0.5),
        'ffn_w_up': nrm((DEPTH, D, 2 * D_FF), D ** -0.5),
        'ffn_conv_w': nrm((DEPTH, CONV_W, D_FF), CONV_W ** -0.5),
        'ffn_w_down': nrm((DEPTH, D_FF, D), D_FF ** -0.5),
        'final_norm_g': 1.0 + nrm((D,), 0.02),
    }


def reference(x, c, mod_w, mod_b, norm_mix_g, norm_ffn_g, ab_w_in, ab_w_out, hgrn_lb_logits,
              hgrn_onorm_g, sconv_w, nsa_w_in, nsa_w_out, nsa_cmp_pos_k, nsa_cmp_pos_v,
              nsa_cmp_w1_k, nsa_cmp_w2_k, nsa_cmp_w1_v, nsa_cmp_w2_v, rel_bias,
              ffn_w_up, ffn_conv_w, ffn_w_down, final_norm_g):
    lower = jnp.cumsum(jax.nn.softmax(hgrn_lb_logits.astype(jnp.float32), axis=0), axis=0)
    c_act = jax.nn.silu(c)
    for l in range(DEPTH):
        mod = c_act @ mod_w[l] + mod_b[l]
        sh1, sc1, g1, sh2, sc2, g2 = jnp.split(mod[:, None, :], 6, axis=-1)
        hm = rmsnorm(x, norm_mix_g[l]) * (1.0 + sc1) + sh1
        j = l // 2
        if l % 2 == 0:
            y = mixer_hgrn2_shortconv(hm, ab_w_in[j], ab_w_out[j], lower[j], hgrn_onorm_g[j], sconv_w[j])
        else:
            y = mixer_nsa(hm, nsa_w_in[j], nsa_w_out[j], nsa_cmp_pos_k[j], nsa_cmp_pos_v[j],
                          nsa_cmp_w1_k[j], nsa_cmp_w2_k[j], nsa_cmp_w1_v[j], nsa_cmp_w2_v[j], rel_bias)
        x = x + g1 * y
        hf = rmsnorm(x, norm_ffn_g[l]) * (1.0 + sc2) + sh2
        x = x + g2 * conv_ffn(hf, ffn_w_up[l], ffn_conv_w[l], ffn_w_down[l])
    return rmsnorm(x, final_norm_g)
```

```python
import numpy as np
from contextlib import ExitStack
import concourse.bass as bass
import concourse.mybir as mybir
from concourse.bass_utils import run_bass_kernel_spmd

F32 = mybir.dt.float32
BF16 = mybir.dt.bfloat16
AF = mybir.ActivationFunctionType
ALU = mybir.AluOpType

D = 1024
S = 4096
NCH = 8
DFF = 2816
NFF = 22
EPS = 1e-6
T = 512
NT = S // T


class Buf:
    __slots__ = ("name", "w", "r", "war")

    def __init__(self, name=""):
        self.name = name
        self.w = {}
        self.r = {}
        self.war = {}


def _merge(d, s):
    for k, v in s.items():
        if d.get(k, 0) < v:
            d[k] = v


class Prog:
    NDS = 8

    def __init__(self, nc, es):
        self.nc = nc
        self.eng = {"pe": nc.tensor, "act": nc.scalar, "dve": nc.vector,
                    "pool": nc.gpsimd, "sp": nc.sync}
        self.semobj = {}
        self.ccnt = {}
        for e in ("pe", "act", "dve", "pool"):
            self.semobj[("c", e)] = es.enter_context(nc.semaphore("s_" + e))
            self.ccnt[e] = 0
        self.dcnt = {}
        self.drr = {}
        for q in ("sp", "pool", "act"):
            self.drr[q] = 0
            for i in range(self.NDS):
                self.semobj[("d", q, i)] = es.enter_context(nc.semaphore("d_%s%d" % (q, i)))
                self.dcnt[(q, i)] = 0
        self.waited = {e: {} for e in self.eng}
        self.ninstr = 0
        self.sig_all = False

    def _wait(self, eng, deps):
        w = self.waited[eng]
        for key, val in deps.items():
            if w.get(key, 0) >= val:
                continue
            self.eng[eng].wait_ge(self.semobj[key], val)
            w[key] = val
            self.ninstr += 1

    def _deps(self, reads, writes, waw=True):
        deps = {}
        for b in reads:
            _merge(deps, b.w)
        for b in writes:
            if b.r:
                b.war = b.r
                b.r = {}
                b.w = {}
            _merge(deps, b.war)
            if waw:
                _merge(deps, b.w)
        return deps

    def op(self, eng, fn, reads=(), writes=(), sig=True):
        deps = self._deps(reads, writes, waw=(eng != "pe"))
        if eng == "pe":
            deps.pop(("c", "pe"), None)
        self._wait(eng, deps)
        ins = fn()
        key = ("c", eng)
        val = self.ccnt[eng] + 1
        if sig:
            ins.then_inc(self.semobj[key], 1)
            self.ccnt[eng] = val
        self.ninstr += 1
        for b in reads:
            if b.r.get(key, 0) < val:
                b.r[key] = val
        for b in writes:
            if b.w.get(key, 0) < val:
                b.w[key] = val
        return ins

    def dma(self, q, out, in_, reads=(), writes=(), **kw):
        i = self.drr[q]
        self.drr[q] = (i + 1) % self.NDS
        key = ("d", q, i)
        prev = self.dcnt[(q, i)]
        deps = self._deps(reads, writes)
        if prev > 0 and deps.get(key, 0) < prev:
            deps[key] = prev
        self._wait(q, deps)
        ins = self.eng[q].dma_start(out=out, in_=in_, **kw)
        val = prev + 16
        ins.then_inc(self.semobj[key], 16)
        self.dcnt[(q, i)] = val
        self.ninstr += 1
        for b in reads:
            if b.r.get(key, 0) < val:
                b.r[key] = val
        for b in writes:
            if b.w.get(key, 0) < val:
                b.w[key] = val
        return ins

    def finish(self, bufs):
        deps = {}
        for b in bufs:
            _merge(deps, b.w)
        for e, v in self.ccnt.items():
            if v > 0:
                deps[("c", e)] = max(deps.get(("c", e), 0), v)
        for (q, i), v in self.dcnt.items():
            if v > 0:
                deps[("d", q, i)] = max(deps.get(("d", q, i), 0), v)
        self._wait("sp", deps)

    def mm(self, out, lhsT, rhs, start, stop, reads, writes, **kw):
        nc = self.nc
        return self.op("pe", lambda: nc.tensor.matmul(out, lhsT, rhs, start=start, stop=stop, **kw),
                       reads=reads, writes=writes, sig=(stop or self.sig_all))

    def act(self, out, in_, func, reads, writes, **kw):
        nc = self.nc
        return self.op("act", lambda: nc.scalar.activation(out=out, in_=in_, func=func, **kw),
                       reads=reads, writes=writes)

    def tt(self, eng, out, in0, in1, op, reads, writes):
        e = self.eng[eng]
        return self.op(eng, lambda: e.tensor_tensor(out=out, in0=in0, in1=in1, op=op),
                       reads=reads, writes=writes)

    def ts(self, eng, out, in0, s1, s2, op0, op1, reads, writes):
        e = self.eng[eng]
        if s2 is None:
            return self.op(eng, lambda: e.tensor_scalar(out=out, in0=in0, scalar1=s1, scalar2=None, op0=op0),
                           reads=reads, writes=writes)
        return self.op(eng, lambda: e.tensor_scalar(out=out, in0=in0, scalar1=s1, scalar2=s2, op0=op0, op1=op1),
                       reads=reads, writes=writes)

    def stt(self, out, in0, scalar, in1, op0, op1, reads, writes):
        nc = self.nc
        return self.op("dve", lambda: nc.vector.scalar_tensor_tensor(out=out, in0=in0, scalar=scalar, in1=in1,
                                                                    op0=op0, op1=op1),
                       reads=reads, writes=writes)


class Ctx:
    def __init__(self, nc, es):
        self.nc = nc
        self.es = es
        self.n = 0

    def sb(self, shape, dt, name=None):
        self.n += 1
        t = self.es.enter_context(self.nc.sbuf_tensor(name or ("sb%d" % self.n), list(shape), dt))
        return t, Buf(name or "sb")

    def ps(self, shape, dt, name=None):
        self.n += 1
        t = self.es.enter_context(self.nc.psum_tensor(name or ("ps%d" % self.n), list(shape), dt))
        return t, Buf(name or "ps")


def emit_norm_mod(P, K, xt, xb, a_ap, sh_ap, out, outb, ps, psb, pbufs=()):
    nc = P.nc
    sq, sqb = K["sq"]
    rstd, rb = K["rstd"]
    tmp, tb = K["tmp"]
    ones, ob = K["ones"]
    epsb, eb = K["eps"]
    P.act(sq[:, :, :], xt[:, :, :], AF.Square, [xb], [sqb])
    for c in range(NCH):
        P.mm(ps[:, :], ones[:, :], sq[:, c, :], c == 0, c == NCH - 1, [ob, sqb], [psb])
    P.act(rstd[:, :], ps[:, :], AF.Sqrt, [psb, eb], [rb], scale=1.0 / D, bias=epsb[:, 0:1])
    P.op("dve", lambda: nc.vector.reciprocal(out=rstd[:, :], in_=rstd[:, :]), [rb], [rb])
    for c in range(NCH):
        P.tt("dve", tmp[:, c, :], xt[:, c, :], rstd[:, :], ALU.mult, [xb, rb], [tb])
        if sh_ap is None:
            P.ts("pool", out[:, c, :], tmp[:, c, :], a_ap[:, c:c + 1], None, ALU.mult, None, [tb] + list(pbufs), [outb])
        else:
            P.ts("pool", out[:, c, :], tmp[:, c, :], a_ap[:, c:c + 1], sh_ap[:, c:c + 1], ALU.mult, ALU.add,
                 [tb] + list(pbufs), [outb])


def alloc_common(cx, P):
    nc = P.nc
    K = {}
    K["sq"] = cx.sb([128, NCH, T], BF16, "sq")
    K["rstd"] = cx.sb([128, T], F32, "rstd")
    K["tmp"] = cx.sb([128, NCH, T], F32, "tmpn")
    K["ones"] = cx.sb([128, 128], BF16, "ones")
    K["eps"] = cx.sb([128, 1], F32, "epsc")
    ones, ob = K["ones"]
    P.op("dve", lambda: nc.vector.memset(ones[:, :], 1.0), [], [ob])
    e, eb = K["eps"]
    P.op("dve", lambda: nc.vector.memset(e[:, :], EPS), [], [eb])
    return K


def build_prologue():
    nc = bass.Bass("TRN2", target_bir_lowering=False)
    cT = nc.dram_tensor("cT", [128, NCH], F32, kind="ExternalInput").ap()
    modw = nc.dram_tensor("modw", [2, 128, NCH, 6 * D], F32, kind="ExternalInput").ap()
    modb = nc.dram_tensor("modb", [128, 2, 48], F32, kind="ExternalInput").ap()
    modv = nc.dram_tensor("modv", [128, 2, 48], F32, kind="ExternalOutput").ap()
    with ExitStack() as es:
        P = Prog(nc, es)
        cx = Ctx(nc, es)
        ct, cb = cx.sb([128, NCH], F32, "ct")
        sg, sgb = cx.sb([128, NCH], F32, "sg")
        mb, mbb = cx.sb([128, 2, 48], F32, "mb")
        mv, mvb = cx.sb([128, 2, 48], F32, "mv")
        wbuf = [cx.sb([128, NCH, 512], F32, "wb%d" % i) for i in range(2)]
        pst, psb = cx.ps([128, 512], F32, "pp")
        P.dma("sp", ct[:, :], cT[:, :], [], [cb])
        P.dma("sp", mb[:, :, :], modb[:, :, :], [], [mbb])
        P.act(sg[:, :], ct[:, :], AF.Silu, [cb], [sgb])
        k = 0
        for l in range(2):
            for blk in range(12):
                w, wb = wbuf[k % 2]
                k += 1
                P.dma("sp", w[:, :, :], modw[l, :, :, blk * 512:(blk + 1) * 512], [], [wb])
                for j in range(4):
                    col = blk * 4 + j
                    for c in range(NCH):
                        P.mm(pst[:, col:col + 1], w[:, c, j * 128:(j + 1) * 128], sg[:, c:c + 1],
                             c == 0, c == NCH - 1, [wb, sgb], [psb])
            P.tt("dve", mv[:, l, :], pst[:, 0:48], mb[:, l, :], ALU.add, [psb, mbb], [mvb])
        ob = Buf("out")
        P.dma("sp", modv[:, :, :], mv[:, :, :], [mvb], [ob])
        P.finish([ob])
    return nc


def build_ffn(final):
    nc = bass.Bass("TRN2", target_bir_lowering=False)
    xin = nc.dram_tensor("xin", [128, NCH, S], F32, kind="ExternalInput").ap()
    modv = nc.dram_tensor("modv", [128, 48], F32, kind="ExternalInput").ap()
    ng = nc.dram_tensor("ng", [128, NCH], F32, kind="ExternalInput").ap()
    fg = nc.dram_tensor("fg", [128, NCH], F32, kind="ExternalInput").ap()
    wup = nc.dram_tensor("wup", [128, NCH, 2 * DFF], F32, kind="ExternalInput").ap()
    wdn = nc.dram_tensor("wdn", [128, NFF, D], F32, kind="ExternalInput").ap()
    cw = nc.dram_tensor("cw", [128, NFF, 3], F32, kind="ExternalInput").ap()
    xout = nc.dram_tensor("xout", [128, NCH, S], F32, kind="ExternalOutput").ap()
    with ExitStack() as es:
        P = Prog(nc, es)
        cx = Ctx(nc, es)
        K = alloc_common(cx, P)
        mv, mvb = cx.sb([128, 48], F32, "mv")
        ngt, ngb = cx.sb([128, NCH], F32, "ngt")
        fgt, fgb = cx.sb([128, NCH], F32, "fgt")
        a2, a2b = cx.sb([128, NCH], F32, "a2")
        cwt, cwb = cx.sb([128, NFF, 3], F32, "cwt")
        wd, wdb = cx.sb([128, NFF, D], BF16, "wd")
        wu = [cx.sb([128, NCH, 512], BF16, "wu%d" % i) for i in range(3)]
        xts = [cx.sb([128, NCH, T], F32, "xt%d" % i) for i in range(2)]
        hT, hb = cx.sb([128, NCH, T], BF16, "hT")
        aT, ab = cx.sb([128, NFF, T], BF16, "aT")
        gbuf, gbb = cx.sb([128, NFF, 2], F32, "ghalo")
        gsb = [cx.sb([128, T + 2], F32, "gsb%d" % i) for i in range(2)]
        c1 = [cx.sb([128, T], F32, "c1_%d" % i) for i in range(2)]
        sl = [cx.sb([128, T], F32, "sl_%d" % i) for i in range(2)]
        psn, psnb = cx.ps([128, T], F32, "psn")
        psg = [cx.ps([128, T], F32, "psg%d" % i) for i in range(2)]
        psu = [cx.ps([128, T], F32, "psu%d" % i) for i in range(2)]
        psy = [cx.ps([128, T], F32, "psy%d" % i) for i in range(2)]

        P.dma("sp", mv[:, :], modv[:, :], [], [mvb])
        P.dma("sp", ngt[:, :], ng[:, :], [], [ngb])
        P.dma("sp", fgt[:, :], fg[:, :], [], [fgb])
        P.dma("sp", cwt[:, :, :], cw[:, :, :], [], [cwb])
        for j in range(NFF):
            P.dma("pool", wd[:, j, :], wdn[:, j, :], [], [wdb])
        P.op("dve", lambda: nc.vector.memset(gbuf[:, :, :], 0.0), [], [gbb])
        P.stt(a2[:, :], mv[:, 32:40], 1.0, ngt[:, :], ALU.add, ALU.mult, [mvb, ngb], [a2b])
        sh2 = mv[:, 24:32]
        g2 = mv[:, 40:48]
        ob = Buf("xout")
        wk = 0
        for tt in range(NT):
            xt, xb = xts[tt % 2]
            P.dma("sp", xt[:, :, :], xin[:, :, tt * T:(tt + 1) * T], [], [xb])
            emit_norm_mod(P, K, xt, xb, a2, sh2, hT, hb, psn, psnb, [a2b, mvb])
            for jb in range(NFF // 2):
                w, wb = wu[wk % 3]
                wk += 1
                P.dma("pool", w[:, :, 0:256], wup[:, :, jb * 256:(jb + 1) * 256], [], [wb])
                P.dma("pool", w[:, :, 256:512], wup[:, :, DFF + jb * 256:DFF + (jb + 1) * 256], [], [wb])
                for jj in range(2):
                    j = jb * 2 + jj
                    pg, pgb = psg[j % 2]
                    pu, pub = psu[j % 2]
                    g_s, g_b = gsb[j % 2]
                    c_s, c_b = c1[j % 2]
                    s_s, s_b = sl[j % 2]
                    for c in range(NCH):
                        P.mm(pg[:, :], w[:, c, jj * 128:(jj + 1) * 128], hT[:, c, :], c == 0, c == NCH - 1,
                             [wb, hb], [pgb])
                    for c in range(NCH):
                        P.mm(pu[:, :], w[:, c, 256 + jj * 128:256 + (jj + 1) * 128], hT[:, c, :], c == 0,
                             c == NCH - 1, [wb, hb], [pub])
                    P.op("pool", lambda: nc.gpsimd.tensor_copy(out=g_s[:, 0:2], in_=gbuf[:, j, :]), [gbb], [g_b])
                    P.act(g_s[:, 2:T + 2], pg[:, :], AF.Copy, [pgb], [g_b])
                    P.op("pool", lambda: nc.gpsimd.tensor_copy(out=gbuf[:, j, :], in_=g_s[:, T:T + 2]), [g_b], [gbb])
                    P.ts("dve", c_s[:, :], g_s[:, 2:T + 2], cwt[:, j, 2:3], None, ALU.mult, None, [g_b, cwb], [c_b])
                    P.stt(c_s[:, :], g_s[:, 1:T + 1], cwt[:, j, 1:2], c_s[:, :], ALU.mult, ALU.add, [g_b, cwb, c_b], [c_b])
                    P.stt(c_s[:, :], g_s[:, 0:T], cwt[:, j, 0:1], c_s[:, :], ALU.mult, ALU.add, [g_b, cwb, c_b], [c_b])
                    P.act(s_s[:, :], c_s[:, :], AF.Silu, [c_b], [s_b])
                    P.tt("dve", aT[:, j, :], pu[:, :], s_s[:, :], ALU.mult, [pub, s_b], [ab])
            for n in range(NCH):
                py, pyb = psy[n % 2]
                for j in range(NFF):
                    P.mm(py[:, :], wd[:, j, n * 128:(n + 1) * 128], aT[:, j, :], j == 0, j == NFF - 1,
                         [wdb, ab], [pyb])
                P.stt(xt[:, n, :], py[:, :], g2[:, n:n + 1], xt[:, n, :], ALU.mult, ALU.add, [pyb, mvb, xb], [xb])
            if final:
                emit_norm_mod(P, K, xt, xb, fgt, None, xt, xb, psn, psnb, [fgb])
            P.dma("sp", xout[:, :, tt * T:(tt + 1) * T], xt[:, :, :], [xb], [ob])
        P.finish([ob])
    print("ffn ninstr", P.ninstr)
    return nc


def build_hgrn():
    nc = bass.Bass("TRN2", target_bir_lowering=False)
    xin = nc.dram_tensor("xin", [128, NCH, S], F32, kind="ExternalInput").ap()
    modv = nc.dram_tensor("modv", [128, 48], F32, kind="ExternalInput").ap()
    ng = nc.dram_tensor("ng", [128, NCH], F32, kind="ExternalInput").ap()
    win = nc.dram_tensor("win", [128, NCH, 3584], F32, kind="ExternalInput").ap()
    wout = nc.dram_tensor("wout", [128, NCH, D], F32, kind="ExternalInput").ap()
    lbl = nc.dram_tensor("lbl", [128, 2, 4], F32, kind="ExternalInput").ap()
    ogd = nc.dram_tensor("ogd", [128, 1], F32, kind="ExternalInput").ap()
    scw = nc.dram_tensor("scw", [128, 4, 3], F32, kind="ExternalInput").ap()
    rmaskd = nc.dram_tensor("rmaskd", [128, T], F32, kind="ExternalInput").ap()
    bmaskd = nc.dram_tensor("bmaskd", [128, 128], F32, kind="ExternalInput").ap()
    identd = nc.dram_tensor("identd", [128, 128], F32, kind="ExternalInput").ap()
    xout = nc.dram_tensor("xout", [128, NCH, S], F32, kind="ExternalOutput").ap()
    with ExitStack() as es:
        P = Prog(nc, es)
        cx = Ctx(nc, es)
        K = alloc_common(cx, P)
        ones, onesb = K["ones"]
        epsc, epsb = K["eps"]
        mv, mvb = cx.sb([128, 48], F32, "mv")
        ngt, ngb = cx.sb([128, NCH], F32, "ngt")
        a1, a1b = cx.sb([128, NCH], F32, "a1")
        lbt, lbtb = cx.sb([128, 2, 4], F32, "lbt")
        lb, lbb = cx.sb([128, 4], F32, "lb")
        oml, omlb = cx.sb([128, 4], F32, "oml")
        og, ogb = cx.sb([128, 1], F32, "og")
        scwt, scwb = cx.sb([128, 4, 3], F32, "scwt")
        rmask, rmb = cx.sb([128, T], F32, "rmask")
        bmask, bmb = cx.sb([128, 128], F32, "bmask")
        ident, idb = cx.sb([128, 128], BF16, "ident")
        wi, wib = cx.sb([128, NCH, 3584], BF16, "wi")
        wo, wob = cx.sb([128, NCH, D], BF16, "wo")
        xts = [cx.sb([128, NCH, T], F32, "xt%d" % i) for i in range(2)]
        hT, hb = cx.sb([128, NCH, T], BF16, "hT")
        vtok, vtb = cx.sb([128, 4, 512], BF16, "vtok")
        cat, catb = cx.sb([128, NCH, T], BF16, "cat")
        Sst = [cx.sb([128, 128], F32, "S%d" % h) for h in range(4)]
        Sbf = [cx.sb([128, 128], BF16, "Sbf%d" % h) for h in range(4)]
        uh, uhb = cx.sb([128, 4, 2], F32, "uh")
        W = []
        for i in range(1):
            d = {}
            for nm in ("fg", "lf", "bt", "eb", "enb", "omf", "sgl", "osb", "rs", "o1"):
                d[nm] = cx.sb([128, T], F32, "%s%d" % (nm, i))
            for nm in ("qp", "kp", "osq"):
                d[nm] = cx.sb([128, T], BF16, "%s%d" % (nm, i))
            d["kptok"] = cx.sb([128, 4, 128], BF16, "kptok%d" % i)
            W.append(d)
        attn = [cx.sb([128, 128], BF16, "attn%d" % i) for i in range(2)]
        tmpS = [cx.sb([128, 128], F32, "tmpS%d" % i) for i in range(2)]
        ubuf = [cx.sb([128, T + 2], F32, "ubuf%d" % i) for i in range(2)]
        svs = [cx.sb([128, T], F32, "svs%d" % i) for i in range(2)]
        cv = [cx.sb([128, T], F32, "cv%d" % i) for i in range(2)]
        psn, psnb = cx.ps([128, T], F32, "psn")
        pp = [cx.ps([128, T], F32, "pp%d" % i) for i in range(3)]
        psA, psAb = cx.ps([128, T], F32, "psA")
        pso, psob = cx.ps([128, T], F32, "pso")
        psU, psUb = cx.ps([128, T], F32, "psU")
        psT, psTb = cx.ps([128, 4, 128], BF16, "psT")

        P.dma("sp", mv[:, :], modv[:, :], [], [mvb])
        P.dma("sp", ngt[:, :], ng[:, :], [], [ngb])
        P.dma("sp", lbt[:, :, :], lbl[:, :, :], [], [lbtb])
        P.dma("sp", og[:, :], ogd[:, :], [], [ogb])
        P.dma("sp", scwt[:, :, :], scw[:, :, :], [], [scwb])
        P.dma("sp", rmask[:, :], rmaskd[:, :], [], [rmb])
        P.dma("sp", bmask[:, :], bmaskd[:, :], [], [bmb])
        P.dma("pool", ident[:, :], identd[:, :], [], [idb])
        for c in range(NCH):
            P.dma("pool", wi[:, c, 0:1792], win[:, c, 0:1792], [], [wib])
            P.dma("pool", wi[:, c, 1792:3584], win[:, c, 1792:3584], [], [wib])
            P.dma("pool", wo[:, c, :], wout[:, c, :], [], [wob])
        for h in range(4):
            P.op("dve", lambda: nc.vector.memset(Sst[h][0][:, :], 0.0), [], [Sst[h][1]])
            P.op("dve", lambda: nc.vector.memset(Sbf[h][0][:, :], 0.0), [], [Sbf[h][1]])
        P.op("dve", lambda: nc.vector.memset(uh[:, :, :], 0.0), [], [uhb])
        P.stt(a1[:, :], mv[:, 8:16], 1.0, ngt[:, :], ALU.add, ALU.mult, [mvb, ngb], [a1b])
        sh1 = mv[:, 0:8]
        g1 = mv[:, 16:24]
        P.tt("dve", lb[:, :], lbt[:, 0, :], lbt[:, 1, :], ALU.subtract, [lbtb], [lbb])
        P.act(lb[:, :], lb[:, :], AF.Sigmoid, [lbb], [lbb])
        P.ts("dve", oml[:, :], lb[:, :], -1.0, 1.0, ALU.mult, ALU.add, [lbb], [omlb])
        ob = Buf("xout")
        ppk = [0]

        def proj(col):
            ps_, psb_ = pp[ppk[0] % 3]
            ppk[0] += 1
            for c in range(NCH):
                P.mm(ps_[:, :], wi[:, c, col:col + 128], hT[:, c, :], c == 0, c == NCH - 1, [wib, hb], [psb_])
            return ps_, psb_

        for tt in range(NT):
            xt, xb = xts[tt % 2]
            P.dma("sp", xt[:, :, :], xin[:, :, tt * T:(tt + 1) * T], [], [xb])
            emit_norm_mod(P, K, xt, xb, a1, sh1, hT, hb, psn, psnb, [a1b, mvb])
            for sub in range(4):
                ps_, psb_ = pp[ppk[0] % 3]
                ppk[0] += 1
                for c in range(NCH):
                    P.mm(ps_[:, :], hT[:, c, sub * 128:(sub + 1) * 128], wi[:, c, 1024:1536], c == 0,
                         c == NCH - 1, [wib, hb], [psb_])
                P.act(vtok[:, sub, :], ps_[:, :], AF.Copy, [psb_], [vtb])
            for h in range(4):
                w = W[0]
                fgt, fgb_ = w["fg"]
                lf, lfb = w["lf"]
                bt, btb = w["bt"]
                eb, ebb = w["eb"]
                enb, enbb = w["enb"]
                omf, omfb = w["omf"]
                sgl, sglb = w["sgl"]
                osb, osbb = w["osb"]
                rs, rsb = w["rs"]
                o1, o1b = w["o1"]
                qp, qpb = w["qp"]
                kp, kpb = w["kp"]
                osq, osqb = w["osq"]
                kptok, kptb = w["kptok"]
                Sh, Shb = Sst[h]
                Sb, Sbb = Sbf[h]
                psf, psfb = proj(512 + h * 128)
                P.act(fgt[:, :], psf[:, :], AF.Sigmoid, [psfb], [fgb_])
                P.ts("dve", fgt[:, :], fgt[:, :], oml[:, h:h + 1], lb[:, h:h + 1], ALU.mult, ALU.add,
                     [fgb_, omlb, lbb], [fgb_])
                P.act(lf[:, :], fgt[:, :], AF.Ln, [fgb_], [lfb])
                P.op("dve", lambda: nc.vector.tensor_tensor_scan(out=bt[:, :], data0=rmask[:, :], data1=lf[:, :],
                                                                  initial=0.0, op0=ALU.mult, op1=ALU.add),
                     [rmb, lfb], [btb])
                P.act(eb[:, :], bt[:, :], AF.Exp, [btb], [ebb])
                P.act(enb[:, :], bt[:, :], AF.Exp, [btb], [enbb], scale=-1.0)
                psq, psqb = proj(h * 128)
                P.tt("dve", qp[:, :], psq[:, :], eb[:, :], ALU.mult, [psqb, ebb], [qpb])
                P.ts("pool", omf[:, :], fgt[:, :], -1.0, 1.0, ALU.mult, ALU.add, [fgb_], [omfb])
                P.tt("pool", kp[:, :], omf[:, :], enb[:, :], ALU.mult, [omfb, enbb], [kpb])
                for pr in range(4):
                    P.op("pe", lambda: nc.tensor.transpose(psT[:, pr, :], kp[:, pr * 128:(pr + 1) * 128], ident[:, :]),
                         [kpb, idb], [psTb])
                P.op("dve", lambda: nc.vector.tensor_copy(out=kptok[:, :, :], in_=psT[:, :, :]), [psTb], [kptb])
                psg, psgb = proj(1536 + h * 128)
                P.act(sgl[:, :], psg[:, :], AF.Silu, [psgb], [sglb])
                for pr in range(4):
                    cs = slice(pr * 128, (pr + 1) * 128)
                    at, atb = attn[pr % 2]
                    P.mm(psA[:, 0:128], kp[:, cs], qp[:, cs], True, True, [kpb, qpb], [psAb])
                    P.tt("dve", at[:, :], psA[:, 0:128], bmask[:, :], ALU.mult, [psAb, bmb], [atb])
                    P.mm(pso[:, cs], vtok[:, pr, h * 128:(h + 1) * 128], at[:, :], True, False, [vtb, atb], [psob])
                    for half in range(2):
                        c0 = pr * 128 + half * 64
                        rows = slice(half * 64, half * 64 + 64)
                        tS, tSb = tmpS[half]
                        P.mm(pso[:, c0:c0 + 64], Sb[:, :], qp[:, c0:c0 + 64], False, half == 1 and True,
                             [Sbb, qpb], [psob])
                        P.mm(psU[:, 0:128], kptok[rows, pr, :], vtok[rows, pr, h * 128:(h + 1) * 128], True, True,
                             [kptb, vtb], [psUb])
                        P.tt("dve", tS[:, :], psU[:, 0:128], Sh[:, :], ALU.add, [psUb, Shb], [tSb])
                        P.ts("dve", Sh[:, :], tS[:, :], eb[:, c0 + 63:c0 + 64], None, ALU.mult, None,
                             [tSb, ebb], [Shb])
                        P.act(Sb[:, :], tS[:, :], AF.Copy, [tSb, ebb], [Sbb], scale=eb[:, c0 + 63:c0 + 64])
                P.act(osb[:, :], pso[:, :], AF.Copy, [psob], [osbb])
                P.act(osq[:, :], pso[:, :], AF.Square, [psob], [osqb])
                P.mm(psA[:, :], ones[:, :], osq[:, :], True, True, [onesb, osqb], [psAb])
                P.act(rs[:, :], psA[:, :], AF.Sqrt, [psAb, epsb], [rsb], scale=1.0 / 128, bias=epsc[:, 0:1])
                P.op("dve", lambda: nc.vector.reciprocal(out=rs[:, :], in_=rs[:, :]), [rsb], [rsb])
                P.tt("dve", o1[:, :], osb[:, :], rs[:, :], ALU.mult, [osbb, rsb], [o1b])
                P.stt(cat[:, h, :], o1[:, :], og[:, 0:1], sgl[:, :], ALU.mult, ALU.mult, [o1b, ogb, sglb], [catb])
            for j in range(4):
                u_s, u_b = ubuf[j % 2]
                sv_s, sv_b = svs[j % 2]
                c_s, c_b = cv[j % 2]
                psgc, psgcb = proj(2560 + j * 128)
                pssv, pssvb = proj(3072 + j * 128)
                psgb_, psgbb = proj(2048 + j * 128)
                P.act(sv_s[:, :], pssv[:, :], AF.Copy, [pssvb], [sv_b])
                P.op("pool", lambda: nc.gpsimd.tensor_copy(out=u_s[:, 0:2], in_=uh[:, j, :]), [uhb], [u_b])
                P.tt("dve", u_s[:, 2:T + 2], psgc[:, :], sv_s[:, :], ALU.mult, [psgcb, sv_b], [u_b])
                P.op("pool", lambda: nc.gpsimd.tensor_copy(out=uh[:, j, :], in_=u_s[:, T:T + 2]), [u_b], [uhb])
                P.ts("dve", c_s[:, :], u_s[:, 2:T + 2], scwt[:, j, 2:3], None, ALU.mult, None, [u_b, scwb], [c_b])
                P.stt(c_s[:, :], u_s[:, 1:T + 1], scwt[:, j, 1:2], c_s[:, :], ALU.mult, ALU.add, [u_b, scwb, c_b], [c_b])
                P.stt(c_s[:, :], u_s[:, 0:T], scwt[:, j, 0:1], c_s[:, :], ALU.mult, ALU.add, [u_b, scwb, c_b], [c_b])
                P.tt("dve", cat[:, 4 + j, :], psgb_[:, :], c_s[:, :], ALU.mult, [psgbb, c_b], [catb])
            for n in range(NCH):
                py, pyb = pp[ppk[0] % 3]
                ppk[0] += 1
                for k in range(NCH):
                    P.mm(py[:, :], wo[:, k, n * 128:(n + 1) * 128], cat[:, k, :], k == 0, k == NCH - 1,
                         [wob, catb], [pyb])
                P.stt(xt[:, n, :], py[:, :], g1[:, n:n + 1], xt[:, n, :], ALU.mult, ALU.add, [pyb, mvb, xb], [xb])
            P.dma("sp", xout[:, :, tt * T:(tt + 1) * T], xt[:, :, :], [xb], [ob])
        P.finish([ob])
    print("hgrn ninstr", P.ninstr)
    return nc


NEG = -30000.0
LB = 8192
LW = 2048


def build_nsa():
    nc = bass.Bass("TRN2", target_bir_lowering=False)
    xin = nc.dram_tensor("xin", [128, NCH, S], F32, kind="ExternalInput").ap()
    modv = nc.dram_tensor("modv", [128, 48], F32, kind="ExternalInput").ap()
    ng = nc.dram_tensor("ng", [128, NCH], F32, kind="ExternalInput").ap()
    win = nc.dram_tensor("win", [128, NCH, 11 * 256], F32, kind="ExternalInput").ap()
    wout = nc.dram_tensor("wout", [128, NCH, D], F32, kind="ExternalInput").ap()
    w1d = nc.dram_tensor("w1d", [128, 32, 256], F32, kind="ExternalInput").ap()
    w2kd = nc.dram_tensor("w2kd", [128, 2, 2, 128], F32, kind="ExternalInput").ap()
    w2vd = nc.dram_tensor("w2vd", [128, 2, 64], F32, kind="ExternalInput").ap()
    posd = nc.dram_tensor("posd", [128, 32], F32, kind="ExternalInput").ap()
    rbxd = nc.dram_tensor("rbxd", [33, 128], F32, kind="ExternalInput").ap()
    rb31d = nc.dram_tensor("rb31d", [1, 16], F32, kind="ExternalInput")
    ohsd = nc.dram_tensor("ohsd", [33, LB], F32, kind="ExternalInput").ap()
    ohwd = nc.dram_tensor("ohwd", [33, LW], F32, kind="ExternalInput").ap()
    hmd = nc.dram_tensor("hmd", [128, 1408], F32, kind="ExternalInput").ap()
    wexd = nc.dram_tensor("wexd", [64, S], F32, kind="ExternalInput").ap()
    c2sd = nc.dram_tensor("c2sd", [128, 2, 65], F32, kind="ExternalInput").ap()
    fmaskd = nc.dram_tensor("fmaskd", [128, 128], F32, kind="ExternalInput").ap()
    identd = nc.dram_tensor("identd", [128, 128], F32, kind="ExternalInput").ap()
    antid = nc.dram_tensor("antid", [128, 128], F32, kind="ExternalInput").ap()
    xout = nc.dram_tensor("xout", [128, NCH, S], F32, kind="ExternalOutput").ap()
    btab = nc.dram_tensor("btab", [16, LB], BF16)
    bwtab = nc.dram_tensor("bwtab", [16, LW], BF16)
    gdram = nc.dram_tensor("gdram", [48, T], F32)
    with ExitStack() as es:
        P = Prog(nc, es)
        P.sig_all = True
        cx = Ctx(nc, es)
        ones, onesb = cx.sb([128, 128], BF16, "ones")
        epsc, epsb = cx.sb([128, 1], F32, "epsc")
        P.op("dve", lambda: nc.vector.memset(ones[:, :], 1.0), [], [onesb])
        P.op("dve", lambda: nc.vector.memset(epsc[:, :], EPS), [], [epsb])
        mv, mvb = cx.sb([128, 48], F32, "mv")
        ngt, ngb = cx.sb([128, NCH], F32, "ngt")
        a1, a1b = cx.sb([128, NCH], F32, "a1")
        ident, idb = cx.sb([128, 128], BF16, "ident")
        identf, idfb = cx.sb([128, 128], F32, "identf")
        anti, antb = cx.sb([128, 128], BF16, "anti")
        w1, w1b = cx.sb([128, 32, 256], BF16, "w1")
        w2k, w2kb = cx.sb([128, 2, 2, 128], BF16, "w2k")
        w2v, w2vb = cx.sb([128, 2, 64], BF16, "w2v")
        posT, posb = cx.sb([128, 32], BF16, "posT")
        pbias, pbb = cx.sb([128, 2, 2], F32, "pbias")
        c31, c31b = cx.sb([128, 16], F32, "c31")
        hm, hmb = cx.sb([128, 1408], BF16, "hm")
        wex, wexb = cx.sb([64, S], BF16, "wex")
        c2s, c2sb = cx.sb([128, 2, 65], BF16, "c2s")
        fmask, fmb = cx.sb([128, 128], F32, "fmask")
        KsT, KsTb = cx.sb([128, 2, S], BF16, "KsT")
        KwT, KwTb = cx.sb([128, 2, S], BF16, "KwT")
        Vs, Vsb = cx.sb([128, 32, 2, 3, 64], BF16, "Vs")
        Vw, Vwb = cx.sb([128, 32, 2, 3, 64], BF16, "Vw")
        Vc, Vcb = cx.sb([128, 2, 2, 3, 64], BF16, "Vc")
        KcT, KcTb = cx.sb([128, 4, 256], BF16, "KcT")
        kcvc, kcvcb = cx.sb([128, 4, 16 + T], BF16, "kcvc")
        wst = [cx.sb([128, NCH, 256], BF16, "wst%d" % i) for i in range(2)]
        xc = [cx.sb([128, T], F32, "xc%d" % i) for i in range(2)]
        sqc = [cx.sb([128, T], BF16, "sqc%d" % i) for i in range(2)]
        tmpc = [cx.sb([128, T], F32, "tmpc%d" % i) for i in range(1)]
        rstd, rstdb = cx.sb([128, T], F32, "rstd")
        hT, hb = cx.sb([128, NCH, T], BF16, "hT")
        QT, QTb = cx.sb([128, NCH, T], BF16, "QT")
        GT, GTb = cx.sb([48, T], F32, "GT")
        cat, catb = cx.sb([128, NCH, T], BF16, "cat")
        strips, stripsb = cx.sb([128, 4, 1920], BF16, "strips")
        hc = [cx.sb([128, T], BF16, "hc%d" % i) for i in range(2)]
        PT = [cx.sb([128, T], BF16, "PT%d" % i) for i in range(3)]
        acc = [cx.sb([128, T], F32, "acc%d" % i) for i in range(4)]
        rec, recb = cx.sb([128, T], F32, "rec")
        gt_, gtb_ = cx.sb([128, T], F32, "gatet")
        wg, wgb = cx.sb([128, T], F32, "wg")
        tpr, tprb = cx.sb([128, T], F32, "tpr")
        selT, selTb = cx.sb([64, T], BF16, "selT")
        hsb, hsbb = cx.sb([128, 2, 128], BF16, "hsb")
        vstg, vstgb = cx.sb([32, 4, 64], BF16, "vstg")
        impa, impab = cx.sb([128, 64], F32, "impa")
        tpr_imp, impwb = cx.sb([128, 256], F32, "impw")
        sc8, sc8b = cx.sb([128, 16], F32, "sc8")
        scr, scrb = cx.sb([128, 64], F32, "scr")
        rden, rdenb = cx.sb([128, 1], F32, "rden")
        selb, selbb = cx.sb([128, 64], BF16, "selb")
        tb16, tb16b = cx.sb([33, 512], BF16, "tb16")
        rbx, rbxb = cx.sb([33, 128], BF16, "rbx")
        tbo, tbob = cx.sb([16, 512], BF16, "tbo")
        pss = [cx.ps([128, T], F32, "pss%d" % i) for i in range(3)]
        pso = [cx.ps([128, T], F32, "pso%d" % i) for i in range(2)]
        psm = [cx.ps([128, T], F32, "psm%d" % i) for i in range(2)]
        psT, psTb = cx.ps([128, 4, 128], BF16, "psT")

        def cast_load(dst, dstb, src):
            P.dma("pool", dst, src, [], [dstb])

        P.dma("sp", mv[:, :], modv[:, :], [], [mvb])
        P.dma("sp", ngt[:, :], ng[:, :], [], [ngb])
        P.dma("sp", fmask[:, :], fmaskd[:, :], [], [fmb])
        P.dma("sp", identf[:, :], identd[:, :], [], [idfb])
        P.dma("pool", rbx[:, :], rbxd[:, :], [], [rbxb])
        P.dma("sp", c31[:, :], bass.AP(rb31d, 0, [[0, 128], [1, 16]]), [], [c31b])
        cast_load(ident[:, :], idb, identd[:, :])
        cast_load(anti[:, :], antb, antid[:, :])
        for l0 in range(0, 32, 8):
            cast_load(w1[:, l0:l0 + 8, :], w1b, w1d[:, l0:l0 + 8, :])
        cast_load(w2k[:, :, :, :], w2kb, w2kd[:, :, :, :])
        cast_load(w2v[:, :, :], w2vb, w2vd[:, :, :])
        cast_load(posT[:, :], posb, posd[:, :])
        cast_load(hm[:, :], hmb, hmd[:, :])
        cast_load(wex[:, 0:2048], wexb, wexd[:, 0:2048])
        cast_load(wex[:, 2048:4096], wexb, wexd[:, 2048:4096])
        cast_load(c2s[:, :, :], c2sb, c2sd[:, :, :])
        for t_, tb_ in ((KsT, KsTb), (KwT, KwTb)):
            P.op("pool", lambda: nc.gpsimd.memset(t_[:, :, :], 0.0), [], [tb_])
        for t_, tb_ in ((Vs, Vsb), (Vw, Vwb), (Vc, Vcb)):
            P.op("pool", lambda: nc.gpsimd.memset(t_[:, :, :, :, :], 0.0), [], [tb_])
            P.op("pool", lambda: nc.gpsimd.memset(t_[:, :, :, 1, :], 1.0), [], [tb_])
        P.op("dve", lambda: nc.vector.memset(KcT[:, :, :], 0.0), [], [KcTb])
        P.op("dve", lambda: nc.vector.memset(kcvc[:, :, :], 0.0), [], [kcvcb])
        P.op("dve", lambda: nc.vector.memset(hsb[:, :, :], 0.0), [], [hsbb])
        P.stt(a1[:, :], mv[:, 8:16], 1.0, ngt[:, :], ALU.add, ALU.mult, [mvb, ngb], [a1b])
        sh1 = mv[:, 0:8]
        g1 = mv[:, 16:24]
        btb_ = Buf("btab")
        bwb_ = Buf("bwtab")
        for (tab, tbuf, ohd, L) in ((btab, btb_, ohsd, LB), (bwtab, bwb_, ohwd, LW)):
            for ch in range(L // 512):
                P.dma("pool", tb16[:, :], ohd[:, ch * 512:(ch + 1) * 512], [], [tb16b])
                P.mm(psm[0][0][:, :], rbx[:, :], tb16[:, :], True, True, [rbxb, tb16b], [psm[0][1]])
                P.act(tbo[:, :], psm[0][0][0:16, :], AF.Copy, [psm[0][1]], [tbob])
                P.dma("sp", tab.ap()[:, ch * 512:(ch + 1) * 512], tbo[:, :], [tbob], [tbuf])
        for kv in range(2):
            rows = slice(kv * 64, kv * 64 + 64)
            for ht in range(2):
                for l in range(32):
                    P.mm(psm[1][0][:, kv * 2 + ht:kv * 2 + ht + 1], w1[rows, l, ht * 128:(ht + 1) * 128],
                         posT[rows, l:l + 1], l == 0, l == 31, [w1b, posb], [psm[1][1]])
        P.op("dve", lambda: nc.vector.tensor_copy(out=pbias[:, :, :].rearrange("p a b -> p (a b)"),
                                                   in_=psm[1][0][:, 0:4]), [psm[1][1]], [pbb])
        ob = Buf("xout")
        gdb = Buf("gdram")
        cnt = {"w": 0, "x": 0, "s": 0, "o": 0, "m": 0, "p": 0, "h": 0}

        def wblock(blk):
            w_, wb_ = wst[cnt["w"] % 2]
            cnt["w"] += 1
            P.dma("pool", w_[:, :, :], win[:, :, blk * 256:(blk + 1) * 256], [], [wb_])
            return w_, wb_

        def nxt(pool, key):
            r = pool[cnt[key] % len(pool)]
            cnt[key] += 1
            return r

        def fm_proj(w_, wb_, j, M=128):
            ps_, psb_ = nxt(pss, "s")
            for c in range(NCH):
                P.mm(ps_[0:M, :], w_[:, c, j * 128:j * 128 + M], hT[:, c, :], c == 0, c == NCH - 1,
                     [wb_, hb], [psb_])
            return ps_, psb_

        import os as _os
        _stop = float(_os.environ.get('NSA_STOP', 99))

        class _Stop(Exception):
            pass

        def chk(k):
            if _stop <= k:
                raise _Stop()
        try:
          for tt in range(int(_os.environ.get('NSA_NT', NT))):
              q0 = tt * T
              psn, psnb = nxt(psm, "m")
              for c in range(NCH):
                  x_, xb_ = nxt(xc, "x")
                  s_, sb_ = sqc[c % 2]
                  P.dma("sp", x_[:, :], xin[:, c, q0:q0 + T], [], [xb_])
                  P.act(s_[:, :], x_[:, :], AF.Square, [xb_], [sb_])
                  P.mm(psn[:, :], ones[:, :], s_[:, :], c == 0, c == NCH - 1, [onesb, sb_], [psnb])
              P.act(rstd[:, :], psn[:, :], AF.Sqrt, [psnb, epsb], [rstdb], scale=1.0 / D, bias=epsc[:, 0:1])
              P.op("dve", lambda: nc.vector.reciprocal(out=rstd[:, :], in_=rstd[:, :]), [rstdb], [rstdb])
              for c in range(NCH):
                  x_, xb_ = nxt(xc, "x")
                  t_, tb_ = tmpc[0]
                  P.dma("sp", x_[:, :], xin[:, c, q0:q0 + T], [], [xb_])
                  P.tt("dve", t_[:, :], x_[:, :], rstd[:, :], ALU.mult, [xb_, rstdb], [tb_])
                  P.ts("pool", hT[:, c, :], t_[:, :], a1[:, c:c + 1], sh1[:, c:c + 1], ALU.mult, ALU.add,
                       [tb_, a1b, mvb], [hb])
              chk(1)
              for blk in range(4):
                  w_, wb_ = wblock(blk)
                  for j in range(2):
                      ps_, psb_ = fm_proj(w_, wb_, j)
                      P.act(QT[:, blk * 2 + j, :], ps_[:, :], AF.Copy, [psb_], [QTb], scale=0.125)
              chk(1.1)
              P.op("pool", lambda: nc.gpsimd.tensor_copy(out=kcvc[:, :, 0:16], in_=kcvc[:, :, T:T + 16]),
                   [kcvcb], [kcvcb])
              for blk in (4, 5):
                  w_, wb_ = wblock(blk)
                  for j in range(2):
                      ps_, psb_ = fm_proj(w_, wb_, j)
                      P.act(kcvc[:, (blk - 4) * 2 + j, 16:16 + T], ps_[:, :], AF.Copy, [psb_], [kcvcb])
              chk(1.2)
              for blk, (Kc_, Kcb_) in ((6, (KsT, KsTb)), (7, (KwT, KwTb))):
                  w_, wb_ = wblock(blk)
                  for j in range(2):
                      ps_, psb_ = fm_proj(w_, wb_, j)
                      P.act(Kc_[:, j, q0:q0 + T], ps_[:, :], AF.Copy, [psb_], [Kcb_])
              chk(1.3)
              w_, wb_ = wblock(8)
              ps_, psb_ = fm_proj(w_, wb_, 0)
              P.act(GT[:, :], ps_[0:48, :], AF.Sigmoid, [psb_], [GTb])
              P.dma("sp", gdram.ap()[:, :], GT[:, :], [GTb], [gdb])
              chk(1.4)
              for blk, (Vc_, Vcb_) in ((9, (Vs, Vsb)), (10, (Vw, Vwb))):
                  w_, wb_ = wblock(blk)
                  for sub in range(4):
                      ps_, psb_ = nxt(pss, "s")
                      for c in range(NCH):
                          P.mm(ps_[:, 0:256], hT[:, c, sub * 128:(sub + 1) * 128], w_[:, c, :], c == 0, c == NCH - 1,
                               [wb_, hb], [psb_])
                      P.act(Vc_[:, tt * 4 + sub, :, 0:3:2, :],
                            ps_[:, 0:256].rearrange("p (a b d) -> p a b d", a=2, b=2), AF.Copy, [psb_], [Vcb_])
              chk(2)
              r0 = 1 if tt == 0 else 0
              nr = 32 - r0
              nb0 = 32 * tt - 1 + r0
              for g in range(4):
                  half = g % 2
                  for kv in range(2):
                      rows = slice(kv * 64, kv * 64 + 64)
                      for ht in range(2):
                          ps_, psb_ = nxt(pss, "s")
                          for l in range(32):
                              P.mm(ps_[:, 0:nr], w1[rows, l, ht * 128:(ht + 1) * 128],
                                   kcvc[rows, g, l + 16 * r0:l + 16 * r0 + 16 * (nr - 1) + 1:16],
                                   l == 0, l == 31, [w1b, kcvcb], [psb_])
                          P.act(hsb[:, ht, 0:nr], ps_[:, 0:nr], AF.Silu, [psb_, pbb], [hsbb],
                                bias=pbias[:, kv, ht:ht + 1])
                      if kv == 0:
                          ps_, psb_ = nxt(pss, "s")
                          for ht in range(2):
                              P.mm(ps_[:, 0:nr], w2k[:, ht, half, :], hsb[:, ht, 0:nr], ht == 0, ht == 1,
                                   [w2kb, hsbb], [psb_])
                          hr = slice(half * 64, half * 64 + 64)
                          P.act(KcT[hr, g, nb0:nb0 + nr], ps_[hr, 0:nr], AF.Copy, [psb_], [KcTb])
                      else:
                          ps_, psb_ = nxt(pss, "s")
                          for ht in range(2):
                              P.mm(ps_[:, 0:64], hsb[:, ht, :], w2v[:, ht, :], ht == 0, ht == 1,
                                   [w2vb, hsbb], [psb_])
                          P.act(vstg[0:nr, g, :], ps_[0:nr, 0:64], AF.Copy, [psb_], [vstgb])
              chk(3)
              for g in range(4):
                  a_, blkc = g // 2, (0 if g % 2 == 0 else 2)
                  n = nb0
                  r = 0
                  while r < nr:
                      ntile, p0 = n // 128, n % 128
                      cntr = min(nr - r, 128 - p0)
                      P.dma("sp", Vc[p0:p0 + cntr, ntile, a_, blkc, :], vstg[r:r + cntr, g, :], [vstgb], [Vcb])
                      r += cntr
                      n += cntr
              chk(4)
              for g in range(4):
                  half = g % 2
                  a_ = g // 2
                  hr = slice(half * 64, half * 64 + 64)
                  dr = slice((1 - half) * 64, (1 - half) * 64 + 64)
                  vsl = slice(0, 2) if half == 0 else slice(1, 3)

                  def lhsV(cache, kt):
                      return cache[:, kt, a_, vsl, :].rearrange("p b d -> p (b d)")

                  for i in range(4):
                      h = 4 * g + i
                      P.dma("sp", strips[:, i, :], bass.AP(btab, h * LB + 3584, [[1, 128], [1, 1920]]),
                            [btb_], [stripsb])

                  def finish_branch(br, i, po, pob, first):
                      h = 4 * g + i
                      ac, acb = acc[i]
                      P.ts("dve", rec[hr, :], po[dr, :], 1e-30, None, ALU.max, None, [pob], [recb])
                      P.op("dve", lambda: nc.vector.reciprocal(out=rec[hr, :], in_=rec[hr, :]), [recb], [recb])
                      P.dma("sp", gt_[hr, :], bass.AP(gdram, (br * 16 + h) * T, [[0, 64], [1, T]]), [gdb], [gtb_])
                      P.tt("pool", wg[hr, :], rec[hr, :], gt_[hr, :], ALU.mult, [recb, gtb_], [wgb])
                      if first:
                          P.tt("dve", ac[hr, :], po[hr, :], wg[hr, :], ALU.mult, [pob, wgb], [acb])
                      else:
                          P.tt("dve", tpr[hr, :], po[hr, :], wg[hr, :], ALU.mult, [pob, wgb], [tprb])
                          P.tt("pool", ac[hr, :], ac[hr, :], tpr[hr, :], ALU.add, [acb, tprb], [acb])

                  chk(5)
                  ncmp = 2 if (q0 + T - 1) >= 2079 else 1
                  P.op("dve", lambda: nc.vector.memset(tpr_imp[:, :], 0.0), [], [impwb])
                  imps = [None] * 4
                  for i in range(4):
                      h = 4 * g + i
                      m = a_ * 4 + i
                      po, pob = nxt(pso, "o")
                      pts = []
                      for nt in range(ncmp):
                          hc_, hcb_ = nxt(hc, "h")
                          P.dma("sp", hc_[:, :], bass.AP(btab, h * LB + q0 - 2048 * nt + 2032, [[16, 128], [1, T]]),
                                [btb_], [hcb_])
                          ps_, psb_ = nxt(pss, "s")
                          P.mm(ps_[:, :], KcT[hr, g, nt * 128:(nt + 1) * 128], QT[hr, m, :], True, False,
                               [KcTb, QTb], [psb_])
                          P.mm(ps_[:, :], anti[:, :], hc_[:, :], False, True, [antb, hcb_], [psb_])
                          pt, ptb = nxt(PT, "p")
                          P.act(pt[:, :], ps_[:, :], AF.Exp, [psb_], [ptb])
                          pts.append((pt, ptb))
                      for nt in range(ncmp):
                          pt, ptb = pts[nt]
                          P.mm(po[:, :], lhsV(Vc, nt), pt[:, :], nt == 0, nt == ncmp - 1, [Vcb, ptb], [pob])
                      finish_branch(0, i, po, pob, True)
                      pi_, pib_ = nxt(psm, "m")
                      for sub in range(4):
                          for nt in range(ncmp):
                              pt, ptb = pts[nt]
                              P.mm(pi_[:, sub * 65:sub * 65 + 65], pt[:, sub * 128:(sub + 1) * 128], c2s[:, nt, :],
                                   nt == 0, nt == ncmp - 1, [ptb, c2sb], [pib_])
                      imps[i] = (pi_, pib_)
                      if i % 2 == 1:
                          for i2 in (i - 1, i):
                              pi2, pib2 = imps[i2]
                              for sub in range(4):
                                  P.ts("dve", rden[:, :], pi2[:, sub * 65 + 64:sub * 65 + 65], 1e-30, None, ALU.max, None,
                                       [pib2], [rdenb])
                                  P.op("dve", lambda: nc.vector.reciprocal(out=rden[:, :], in_=rden[:, :]),
                                       [rdenb], [rdenb])
                                  P.stt(tpr_imp[:, sub * 64:(sub + 1) * 64], pi2[:, sub * 65:sub * 65 + 64], rden[:, 0:1],
                                        tpr_imp[:, sub * 64:(sub + 1) * 64], ALU.mult, ALU.add, [pib2, rdenb, impwb], [impwb])
                  chk(6)
                  for sub in range(4):
                      qb = (q0 + sub * 128) // 64
                      P.tt("dve", scr[:, :], tpr_imp[:, sub * 64:(sub + 1) * 64], fmask[:, 64 - qb:128 - qb], ALU.add,
                           [impwb, fmb], [scrb])
                      P.ts("dve", scr[:, 0:1], scr[:, 0:1], 10.0, None, ALU.add, None, [scrb], [scrb])
                      P.op("dve", lambda: nc.vector.max(out=sc8[:, 0:8], in_=scr[:, :]), [scrb], [sc8b])
                      P.op("dve", lambda: nc.vector.match_replace(out=impa[:, :], in_to_replace=sc8[:, 0:8],
                                                                   in_values=scr[:, :], imm_value=-1e30),
                           [scrb, sc8b], [impab])
                      P.op("dve", lambda: nc.vector.max(out=sc8[:, 8:16], in_=impa[:, :]), [impab], [sc8b])
                      P.ts("dve", selb[:, :], scr[:, :], sc8[:, 15:16], None, ALU.is_ge, None, [scrb, sc8b], [selbb])
                      P.ts("dve", selb[:, :], selb[:, :], -1.0, -NEG, ALU.add, ALU.mult, [selbb], [selbb])
                      P.op("pe", lambda: nc.tensor.transpose(psT[0:64, sub, :], selb[:, :], ident[:, :]),
                           [selbb, idb], [psTb])
                  P.op("dve", lambda: nc.vector.tensor_copy(out=selT[:, :],
                                                             in_=psT[0:64, :, :].rearrange("p a b -> p (a b)")),
                       [psTb], [selTb])
                  chk(7)
                  for i in range(4):
                      h = 4 * g + i
                      m = a_ * 4 + i
                      po, pob = nxt(pso, "o")
                      nkt = 4 * tt + 4
                      for kt in range(nkt):
                          mm_ = 4 * tt - kt
                          ps_, psb_ = nxt(pss, "s")
                          P.mm(ps_[:, :], KsT[hr, a_, kt * 128:(kt + 1) * 128], QT[hr, m, :], True, False,
                               [KsTb, QTb], [psb_])
                          u0 = 128 * (min(mm_, 8) + 3)
                          P.mm(ps_[:, :], anti[:, :], strips[:, i, u0:u0 + T], False, False, [antb, stripsb], [psb_])
                          P.mm(ps_[:, :], wex[:, kt * 128:(kt + 1) * 128], selT[:, :], False, True, [wexb, selTb], [psb_])
                          pt, ptb = nxt(PT, "p")
                          P.act(pt[:, :], ps_[:, :], AF.Exp, [psb_], [ptb])
                          P.mm(po[:, :], lhsV(Vs, kt), pt[:, :], kt == 0, kt == nkt - 1, [Vsb, ptb], [pob])
                      finish_branch(1, i, po, pob, False)
                      po, pob = nxt(pso, "o")
                      kts = [kt for kt in range(max(0, 4 * tt - 4), 4 * tt + 4)]
                      for ki, kt in enumerate(kts):
                          mm_ = 4 * tt - kt
                          u0 = 128 * (mm_ + 3)
                          ps_, psb_ = nxt(pss, "s")
                          P.mm(ps_[:, :], KwT[hr, a_, kt * 128:(kt + 1) * 128], QT[hr, m, :], True, False,
                               [KwTb, QTb], [psb_])
                          P.mm(ps_[:, :], anti[:, :], strips[:, i, u0:u0 + T], False, mm_ < 1, [antb, stripsb], [psb_])
                          if mm_ >= 1:
                              P.mm(ps_[:, :], anti[:, :], hm[:, u0:u0 + T], False, True, [antb, hmb], [psb_])
                          pt, ptb = nxt(PT, "p")
                          P.act(pt[:, :], ps_[:, :], AF.Exp, [psb_], [ptb])
                          P.mm(po[:, :], lhsV(Vw, kt), pt[:, :], ki == 0, ki == len(kts) - 1, [Vwb, ptb], [pob])
                      finish_branch(2, i, po, pob, False)
                      P.op("pool", lambda: nc.gpsimd.tensor_copy(out=cat[hr, m, :], in_=acc[i][0][hr, :]),
                           [acc[i][1]], [catb])
              chk(8)
              for nb in range(4):
                  w_, wb_ = wst[cnt["w"] % 2]
                  cnt["w"] += 1
                  P.dma("pool", w_[:, :, :], wout[:, :, nb * 256:(nb + 1) * 256], [], [wb_])
                  for j in range(2):
                      n = nb * 2 + j
                      py, pyb = nxt(pss, "s")
                      for k in range(NCH):
                          P.mm(py[:, :], w_[:, k, j * 128:(j + 1) * 128], cat[:, k, :], k == 0, k == NCH - 1,
                               [wb_, catb], [pyb])
                      x_, xb_ = nxt(xc, "x")
                      P.dma("sp", x_[:, :], xin[:, n, q0:q0 + T], [], [xb_])
                      P.stt(x_[:, :], py[:, :], g1[:, n:n + 1], x_[:, :], ALU.mult, ALU.add, [pyb, mvb, xb_], [xb_])
                      P.dma("sp", xout[:, n, q0:q0 + T], x_[:, :], [xb_], [ob])
        except _Stop:
            pass
        P.finish([ob])
    print("nsa ninstr", P.ninstr)
    return nc


def _kmajor(w, nch):
    n = w.shape[1]
    return np.ascontiguousarray(w.reshape(nch, 128, n).transpose(1, 0, 2))


def _vec(v, nch):
    return np.ascontiguousarray(v.reshape(nch, 128).T)


_CACHE = {}


def _get(name, fn):
    if name not in _CACHE:
        _CACHE[name] = fn()
    return _CACHE[name]


def run_prologue(inp):
    nc = _get("pro", build_prologue)
    modw = np.ascontiguousarray(inp["mod_w"].reshape(2, NCH, 128, 6 * D).transpose(0, 2, 1, 3))
    modb = np.ascontiguousarray(inp["mod_b"].reshape(2, 48, 128).transpose(2, 0, 1))
    maps = [{"cT": _vec(inp["c"][b], NCH), "modw": modw, "modb": modb} for b in range(8)]
    res = run_bass_kernel_spmd(nc, maps, core_ids=list(range(8)))
    return [r["modv"] for r in res.results]


def run_ffn(inp, l, xT, modv, final):
    nc = _get("ffn%d" % int(final), lambda: build_ffn(final))
    wup = _kmajor(inp["ffn_w_up"][l], NCH)
    wdn = _kmajor(inp["ffn_w_down"][l], NFF)
    cw = np.ascontiguousarray(inp["ffn_conv_w"][l].reshape(3, NFF, 128).transpose(2, 1, 0))
    ng = _vec(inp["norm_ffn_g"][l], NCH)
    fg = _vec(inp["final_norm_g"], NCH)
    maps = [{"xin": xT[b], "modv": np.ascontiguousarray(modv[b][:, l, :]), "ng": ng, "fg": fg,
             "wup": wup, "wdn": wdn, "cw": cw} for b in range(8)]
    res = run_bass_kernel_spmd(nc, maps, core_ids=list(range(8)))
    return [r["xout"] for r in res.results]


def to_fm(x):
    return np.ascontiguousarray(x.T.reshape(NCH, 128, x.shape[0]).transpose(1, 0, 2))


def from_fm(xT):
    return np.ascontiguousarray(xT.transpose(1, 0, 2).reshape(D, -1).T)


def hgrn_consts():
    t = np.arange(T)
    rmask = np.broadcast_to((t % 64 != 0).astype(np.float32)[None, :], (128, T)).copy()
    s_ = np.arange(128)[:, None]
    t_ = np.arange(128)[None, :]
    bmask = ((s_ // 64 == t_ // 64) & (s_ <= t_)).astype(np.float32)
    ident = np.eye(128, dtype=np.float32)
    return rmask, bmask, ident


def run_hgrn(inp, xT, modv):
    nc = _get("hgrn", build_hgrn)
    win = _kmajor(inp["ab_w_in"][0], NCH)
    wout = _kmajor(inp["ab_w_out"][0], NCH)
    lbl = np.ascontiguousarray(inp["hgrn_lb_logits"].reshape(2, 4, 128).transpose(2, 0, 1))
    og = np.ascontiguousarray(inp["hgrn_onorm_g"][0].reshape(128, 1))
    scw = np.ascontiguousarray(inp["sconv_w"][0].reshape(3, 4, 128).transpose(2, 1, 0))
    ng = _vec(inp["norm_mix_g"][0], NCH)
    rmask, bmask, ident = hgrn_consts()
    maps = [{"xin": xT[b], "modv": np.ascontiguousarray(modv[b][:, 0, :]), "ng": ng, "win": win, "wout": wout,
             "lbl": lbl, "ogd": og, "scw": scw, "rmaskd": rmask, "bmaskd": bmask, "identd": ident} for b in range(8)]
    res = run_bass_kernel_spmd(nc, maps, core_ids=list(range(8)))
    return [r["xout"] for r in res.results]


def _t5_bucket(n):
    n = np.maximum(n, 0)
    nf = np.maximum(n, 16).astype(np.float32)
    large = 16 + (np.log(nf / np.float32(16)) / np.float32(np.log(64.0)) * np.float32(16)).astype(np.int32)
    return np.where(n < 16, n, np.minimum(large, 31)).astype(np.int64)


def nsa_consts():
    e = np.arange(LB)
    d = e - 4095
    ohs = np.zeros((33, LB), np.float32)
    row = np.where(d < 0, 32, _t5_bucket(d))
    ohs[row, e] = 1.0
    e = np.arange(LW)
    d = e - 511
    ohw = np.zeros((33, LW), np.float32)
    row = np.where((d < 0) | (d >= 512), 32, _t5_bucket(d))
    ohw[row, e] = 1.0
    pp = np.arange(128)[:, None]
    u = np.arange(1408)[None, :]
    hm = np.where(u + pp - 511 >= 512, NEG, 0.0).astype(np.float32)
    wex = (np.arange(S)[None, :] // 64 == np.arange(64)[:, None]).astype(np.float32)
    n_cmp = 255
    c_start = np.arange(n_cmp)[:, None] * 16
    s_start = np.arange(64)[None, :] * 64
    inside = np.clip(np.minimum(c_start + 32, s_start + 64) - np.maximum(c_start, s_start), 0, None)
    c2s_full = np.zeros((256, 65), np.float32)
    c2s_full[:255, :64] = inside / 32.0
    c2s_full[:, 64] = 1.0
    c2s = np.ascontiguousarray(c2s_full.reshape(2, 128, 65).transpose(1, 0, 2))
    v = np.arange(128)[None, :]
    delta = v - 64 - pp // 64
    fmask = np.where(delta > 0, -100.0, np.where((delta == 0) | (delta == -1), 10.0, 0.0)).astype(np.float32)
    ident = np.eye(128, dtype=np.float32)
    anti = np.ascontiguousarray(ident[::-1])
    return dict(ohsd=ohs, ohwd=ohw, hmd=hm, wexd=wex, c2sd=c2s, fmaskd=fmask, identd=ident, antid=anti)


def run_nsa(inp, xT, modv):
    nc = _get("nsa", build_nsa)
    w = inp["nsa_w_in"][0]
    q, gl, kc, vc, ks, vs, kw, vw = np.split(w, np.cumsum([1024, 48, 256, 256, 256, 256, 256]), axis=1)
    cols = []
    head_of = {}
    for m in range(8):
        for half in range(2):
            g = (0 if m < 4 else 2) + half
            i = m % 4
            h = 4 * g + i
            head_of[(m, half)] = h
            cols.append(q[:, h * 64:(h + 1) * 64])
    for g in range(4):
        cols.append(kc[:, g * 64:(g + 1) * 64])
        cols.append(vc[:, g * 64:(g + 1) * 64])
    cols.append(ks)
    cols.append(kw)
    cols.append(gl)
    cols.append(np.zeros((D, 256 - 48), np.float32))
    cols.append(vs)
    cols.append(vw)
    win = _kmajor(np.ascontiguousarray(np.concatenate(cols, axis=1)), NCH)
    wo = inp["nsa_w_out"][0]
    rows = []
    for m in range(8):
        for half in range(2):
            h = head_of[(m, half)]
            rows.append(wo[h * 64:(h + 1) * 64])
    wout = _kmajor(np.ascontiguousarray(np.concatenate(rows, axis=0)), NCH)
    w1k = inp["nsa_cmp_w1_k"][0].reshape(32, 64, 256).transpose(1, 0, 2)
    w1v = inp["nsa_cmp_w1_v"][0].reshape(32, 64, 256).transpose(1, 0, 2)
    w1d = np.ascontiguousarray(np.concatenate([w1k, w1v], axis=0))
    w2k = inp["nsa_cmp_w2_k"][0].reshape(2, 128, 64).transpose(1, 0, 2)
    w2kd = np.zeros((128, 2, 2, 128), np.float32)
    w2kd[:, :, 0, 0:64] = w2k
    w2kd[:, :, 1, 64:128] = w2k
    w2vd = np.ascontiguousarray(inp["nsa_cmp_w2_v"][0].reshape(2, 128, 64).transpose(1, 0, 2))
    posd = np.ascontiguousarray(np.concatenate([inp["nsa_cmp_pos_k"][0].T, inp["nsa_cmp_pos_v"][0].T], axis=0))
    rbx = np.zeros((33, 128), np.float32)
    rbx[:32, :16] = inp["rel_bias"]
    rbx[32, :16] = NEG
    rb31 = np.ascontiguousarray(inp["rel_bias"][31:32])
    ng = _vec(inp["norm_mix_g"][1], NCH)
    C = nsa_consts()
    maps = []
    for b in range(8):
        m = {"xin": xT[b], "modv": np.ascontiguousarray(modv[b][:, 1, :]), "ng": ng, "win": win, "wout": wout,
             "w1d": w1d, "w2kd": w2kd, "w2vd": w2vd, "posd": posd, "rbxd": rbx, "rb31d": rb31}
        m.update(C)
        maps.append(m)
    res = run_bass_kernel_spmd(nc, maps, core_ids=list(range(8)))
    return [r["xout"] for r in res.results]


def kernel(**inputs):
    inp = {k: np.asarray(v) for k, v in inputs.items()}
    x = inp["x"]
    xT = [to_fm(x[b]) for b in range(8)]
    modv = run_prologue(inp)
    xT = run_hgrn(inp, xT, modv)
    xT = run_ffn(inp, 0, xT, modv, False)
    xT = run_nsa(inp, xT, modv)
    xT = run_ffn(inp, 1, xT, modv, True)
    out = np.stack([from_fm(xT[b]) for b in range(8)], axis=0).astype(np.float32)
    return out
```
